# Optimizing a Trainium2 kernel written in Bass

```python
import math
import jax, jax.numpy as jnp
from jax import lax
import numpy as np

D_MODEL = 1024
BATCH = 1
SEQ = 16384
DEPTH = 1

ATTN_HEADS = 8
HEAD_DIM = 128
ATTN_W = ATTN_HEADS * HEAD_DIM
MOBA_BLOCK = 256
MOBA_TOPK = 3
Q_CHUNK = 32
ROPE_DIM = HEAD_DIM // 4
ROPE_THETA = 500000.0
DN_HEADS = 8
DN_DK = 128
DN_DV = 128
DN_QK_W = DN_HEADS * DN_DK
DN_V_W = DN_HEADS * DN_DV
DN_CONV = 4
DN_CHUNK = 64
D_FF = int(math.ceil(8 * D_MODEL / 3 / 256) * 256)
EPS = 1e-6

IN_SIZES = [ATTN_W, ATTN_W, ATTN_W,
            DN_QK_W, DN_QK_W, DN_V_W,
            DN_V_W,
            DN_HEADS, DN_HEADS,
            D_MODEL, D_MODEL]
IN_TOTAL = int(sum(IN_SIZES))
IN_SPLITS = [int(s) for s in np.cumsum(IN_SIZES)[:-1]]

kernel_name = "hybrid_moba_gdn_gated_merge_block"


def rms_norm(x, w):
    xf = x.astype(jnp.float32)
    y = xf * lax.rsqrt(jnp.mean(xf * xf, axis=-1, keepdims=True) + EPS)
    return (y * w.astype(jnp.float32)).astype(x.dtype)


def l2_norm(x):
    return x * lax.rsqrt(jnp.sum(x * x, axis=-1, keepdims=True) + EPS)


def partial_rope(x, pos):
    half = ROPE_DIM // 2
    inv = ROPE_THETA ** (-jnp.arange(half, dtype=jnp.float32) * 2.0 / ROPE_DIM)
    ang = pos.astype(jnp.float32)[:, None] * inv[None, :]
    cos = jnp.cos(ang)[None, :, None, :]
    sin = jnp.sin(ang)[None, :, None, :]
    xr = x[..., :ROPE_DIM].astype(jnp.float32)
    x1, x2 = xr[..., :half], xr[..., half:]
    rot = jnp.concatenate([x1 * cos - x2 * sin, x2 * cos + x1 * sin], axis=-1)
    return jnp.concatenate([rot.astype(x.dtype), x[..., ROPE_DIM:]], axis=-1)


def moba_attention(q, k, v):
    B, S, H, D = q.shape
    nb = -(-S // MOBA_BLOCK)
    s_pad = nb * MOBA_BLOCK
    pad = [(0, 0), (0, s_pad - S), (0, 0), (0, 0)]
    q = jnp.pad(q, pad).transpose(0, 2, 1, 3)
    k = jnp.pad(k, pad).transpose(0, 2, 1, 3)
    v = jnp.pad(v, pad).transpose(0, 2, 1, 3)
    kb = k.reshape(B, H, nb, MOBA_BLOCK, D)
    vb = v.reshape(B, H, nb, MOBA_BLOCK, D)
    k_mean = jnp.mean(kb.astype(jnp.float32), axis=3)
    gate = jnp.einsum('bhsd,bhnd->bhsn', q.astype(jnp.float32), k_mean)
    pos = jnp.arange(s_pad)
    q_blk = pos // MOBA_BLOCK
    past = jnp.arange(nb)[None, :] < q_blk[:, None]
    gate = jnp.where(past[None, None], gate, -jnp.inf)
    k_eff = min(MOBA_TOPK, nb)
    _, top_idx = lax.top_k(gate, k_eff)
    top_valid = jnp.arange(k_eff)[None, :] < q_blk[:, None]
    own = jnp.broadcast_to(q_blk[None, None, :, None], (B, H, s_pad, 1))
    idx = jnp.concatenate([top_idx.astype(jnp.int32), own.astype(jnp.int32)], axis=-1)
    slot_valid = jnp.concatenate([top_valid, jnp.ones((s_pad, 1), bool)], axis=-1)
    ns = k_eff + 1
    nc = s_pad // Q_CHUNK
    qc = q.reshape(B, H, nc, Q_CHUNK, D).transpose(2, 0, 1, 3, 4)
    idxc = idx.reshape(B, H, nc, Q_CHUNK, ns).transpose(2, 0, 1, 3, 4)
    validc = slot_valid.reshape(nc, Q_CHUNK, ns)
    posc = pos.reshape(nc, Q_CHUNK)
    key_off = jnp.arange(MOBA_BLOCK)
    bi = jnp.arange(B)[:, None, None, None]
    hi = jnp.arange(H)[None, :, None, None]
    scale = 1.0 / math.sqrt(D)

    def chunk(args):
        qq, ii, vv, pp = args
        ks = kb[bi, hi, ii]
        vs = vb[bi, hi, ii]
        s = jnp.einsum('bhqd,bhqnkd->bhqnk', qq, ks,
                       preferred_element_type=jnp.float32) * scale
        kpos = ii[..., None] * MOBA_BLOCK + key_off
        mask = vv[None, None, :, :, None] & (kpos <= pp[None, None, :, None, None])
        s = jnp.where(mask, s, -jnp.inf)
        p = jax.nn.softmax(s, axis=(-2, -1))
        return jnp.einsum('bhqnk,bhqnkd->bhqd', p.astype(vs.dtype), vs)

    out = lax.map(chunk, (qc, idxc, validc, posc))
    out = out.transpose(1, 2, 0, 3, 4).reshape(B, H, s_pad, D)[:, :, :S]
    return out.transpose(0, 2, 1, 3)


def causal_depthwise_conv(x, w):
    K, C = w.shape
    return lax.conv_general_dilated(x, w[:, None, :].astype(x.dtype), window_strides=(1,),
                                    padding=[(K - 1, 0)],
                                    dimension_numbers=('NWC', 'WIO', 'NWC'),
                                    feature_group_count=C)


def chunk_gated_delta_rule(q, k, v, beta, g):
    B, S, H, dk = q.shape
    dv = v.shape[-1]
    C = DN_CHUNK
    N = S // C
    q = q * (dk ** -0.5)
    tr = lambda t: t.reshape(B, N, C, H, -1).transpose(0, 3, 1, 2, 4)
    q, k, v = tr(q), tr(k), tr(v)
    beta = beta.reshape(B, N, C, H).transpose(0, 3, 1, 2)
    g = jnp.cumsum(g.reshape(B, N, C, H).transpose(0, 3, 1, 2), axis=-1)
    tril = jnp.tril(jnp.ones((C, C), bool))
    strict = jnp.tril(jnp.ones((C, C), bool), -1)
    eye = jnp.eye(C, dtype=q.dtype)
    gdiff = g[..., :, None] - g[..., None, :]
    decay = jnp.where(tril, jnp.exp(jnp.where(tril, gdiff, 0.0)), 0.0)
    k_beta = k * beta[..., None]
    v_beta = v * beta[..., None]
    L = jnp.where(strict, jnp.einsum('bhncd,bhnjd->bhncj', k_beta, k) * decay, 0.0)
    A = L + eye
    T = lax.linalg.triangular_solve(A, jnp.broadcast_to(eye, A.shape), left_side=True,
                                    lower=True, unit_diagonal=True)
    u = jnp.einsum('bhncj,bhnjd->bhncd', T, v_beta)
    w = jnp.einsum('bhncj,bhnjd->bhncd', T, k_beta * jnp.exp(g)[..., None])
    attn = jnp.where(tril, jnp.einsum('bhncd,bhnjd->bhncj', q, k) * decay, 0.0)
    mv = lambda t: jnp.moveaxis(t, 2, 0)

    def step(state, xs):
        qn, kn, un, wn, gn, an = xs
        v_new = un - jnp.einsum('bhcd,bhde->bhce', wn, state)
        o = (jnp.einsum('bhcd,bhde->bhce', qn * jnp.exp(gn)[..., None], state)
             + jnp.einsum('bhcj,bhje->bhce', an, v_new))
        g_last = gn[..., -1]
        state = (state * jnp.exp(g_last)[..., None, None]
                 + jnp.einsum('bhcd,bhce->bhde', kn * jnp.exp(g_last[..., None] - gn)[..., None], v_new))
        return state, o

    s0 = jnp.zeros((B, H, dk, dv), q.dtype)
    _, o = lax.scan(step, s0, (mv(q), mv(k), mv(u), mv(w), mv(g), mv(attn)))
    return o.transpose(1, 0, 3, 2, 4).reshape(B, S, H, dv)


def gated_deltanet(q, k, v, z, b, a, conv_w, a_log, dt_bias, o_norm_w):
    B, S, _ = q.shape
    dt = q.dtype
    qkv = jax.nn.silu(causal_depthwise_conv(jnp.concatenate([q, k, v], axis=-1), conv_w))
    qkv = qkv.astype(jnp.float32)
    q, k, v = jnp.split(qkv, [DN_QK_W, 2 * DN_QK_W], axis=-1)
    q = l2_norm(q.reshape(B, S, DN_HEADS, DN_DK))
    k = l2_norm(k.reshape(B, S, DN_HEADS, DN_DK))
    v = v.reshape(B, S, DN_HEADS, DN_DV)
    beta = jax.nn.sigmoid(b.astype(jnp.float32))
    g = -jnp.exp(a_log.astype(jnp.float32)) * jax.nn.softplus(
        a.astype(jnp.float32) + dt_bias.astype(jnp.float32))
    o = chunk_gated_delta_rule(q, k, v, beta, g)
    o = rms_norm(o, o_norm_w) * jax.nn.silu(z.astype(jnp.float32).reshape(B, S, DN_HEADS, DN_DV))
    return o.reshape(B, S, DN_V_W).astype(dt)


def setup_inputs(seed: int = 0) -> dict:
    key = jax.random.key(seed)
    ks = jax.random.split(key, 20)
    f32 = jnp.float32
    nrm = lambda k_, shape, fan_in: jax.random.normal(k_, shape, f32) * (fan_in ** -0.5)
    gain = lambda k_, shape: 1.0 + 0.05 * jax.random.normal(k_, shape, f32)
    dt = jnp.exp(jax.random.uniform(ks[14], (DEPTH, DN_HEADS), f32, math.log(1e-3), math.log(1e-1)))
    return {
        "x": jax.random.normal(ks[0], (BATCH, SEQ, D_MODEL), f32),
        "norm_mix_pre": gain(ks[1], (DEPTH, D_MODEL)),
        "w_in": nrm(ks[2], (DEPTH, D_MODEL, IN_TOTAL), D_MODEL),
        "conv_w": nrm(ks[3], (DEPTH, DN_CONV, 2 * DN_QK_W + DN_V_W), DN_CONV),
        "a_log": jnp.log(jax.random.uniform(ks[4], (DEPTH, DN_HEADS), f32, 1.0, 16.0)),
        "dt_bias": jnp.log(jnp.expm1(dt)),
        "o_norm_w": gain(ks[5], (DEPTH, DN_DV)),
        "w_o_attn": nrm(ks[6], (DEPTH, ATTN_W, D_MODEL), ATTN_W),
        "w_o_delta": nrm(ks[7], (DEPTH, DN_V_W, D_MODEL), DN_V_W),
        "w_out": nrm(ks[8], (DEPTH, D_MODEL, D_MODEL), D_MODEL),
        "norm_mix_post": gain(ks[9], (DEPTH, D_MODEL)),
        "norm_ffn_pre": gain(ks[10], (DEPTH, D_MODEL)),
        "w_gate": nrm(ks[11], (DEPTH, D_MODEL, D_FF), D_MODEL),
        "w_up": nrm(ks[12], (DEPTH, D_MODEL, D_FF), D_MODEL),
        "w_down": nrm(ks[13], (DEPTH, D_FF, D_MODEL), D_FF),
        "norm_ffn_post": gain(ks[15], (DEPTH, D_MODEL)),
    }


def reference(x, norm_mix_pre, w_in, conv_w, a_log, dt_bias, o_norm_w, w_o_attn, w_o_delta,
              w_out, norm_mix_post, norm_ffn_pre, w_gate, w_up, w_down, norm_ffn_post):
    B, S, _ = x.shape
    pos = jnp.arange(S)
    for l in range(DEPTH):
        h = rms_norm(x, norm_mix_pre[l])
        proj = h @ w_in[l]
        qa, ka, va, qd, kd, vd, zd, bd, ad, ga, gd = jnp.split(proj, IN_SPLITS, axis=-1)
        qa = partial_rope(qa.reshape(B, S, ATTN_HEADS, HEAD_DIM), pos)
        ka = partial_rope(ka.reshape(B, S, ATTN_HEADS, HEAD_DIM), pos)
        va = va.reshape(B, S, ATTN_HEADS, HEAD_DIM)
        ya = moba_attention(qa, ka, va).reshape(B, S, ATTN_W)
        yd = gated_deltanet(qd, kd, vd, zd, bd, ad, conv_w[l], a_log[l], dt_bias[l], o_norm_w[l])
        merged = (jax.nn.sigmoid(ga) * (ya @ w_o_attn[l])
                  + jax.nn.sigmoid(gd) * (yd @ w_o_delta[l]))
        x = x + rms_norm(merged @ w_out[l], norm_mix_post[l])
        h = rms_norm(x, norm_ffn_pre[l])
        f = (jax.nn.silu(h @ w_gate[l]) * (h @ w_up[l])) @ w_down[l]
        x = x + rms_norm(f, norm_ffn_post[l])
    return x
```

```python
import contextlib
import numpy as np
import ml_dtypes
import concourse.bass as bass
import concourse.mybir as mybir
from concourse.bass_utils import run_bass_kernel_spmd

bf = ml_dtypes.bfloat16

F32 = mybir.dt.float32
BF16 = mybir.dt.bfloat16
AF = mybir.ActivationFunctionType
ALU = mybir.AluOpType
AX = mybir.AxisListType

QUEUES = ['pe', 'act', 'dve', 'pool', 'sp']


class Tok:
    __slots__ = ('w', 'r', 'name', 'excl')

    def __init__(self, name='', excl=False):
        self.w = None
        self.r = []
        self.name = name
        self.excl = excl


class Op:
    __slots__ = ('q', 'fn', 'deps', 'need', 'val', 'sem', 'isdma', 'ndma')


class Prog:
    def __init__(self, nc, stack):
        self.nc = nc
        self.stack = stack
        self.ops = {q: [] for q in QUEUES}
        self.qsem = {q: stack.enter_context(nc.semaphore('qs_' + q)) for q in QUEUES}
        self.fence_pending = {q: [] for q in QUEUES}
        self.dma_sems = {}
        self.dma_since_fence = []
        self.nsem = 0

    def sb(self, name, shape, dt):
        return self.stack.enter_context(self.nc.sbuf_tensor('sb_' + name, shape, dt))

    def ps(self, name, shape, dt):
        return self.stack.enter_context(self.nc.psum_tensor('ps_' + name, shape, dt))

    def _mk(self, q, fn, reads, writes):
        o = Op()
        o.q = q
        o.fn = fn
        o.need = False
        o.isdma = False
        o.ndma = 0
        o.sem = None
        o.val = 0
        deps = []
        ex = [t for t in reads if t.excl]
        if ex:
            reads = [t for t in reads if not t.excl]
            writes = list(writes) + [t for t in ex if t not in writes]
        for t in reads:
            if t.w is not None:
                deps.append(t.w)
        for t in writes:
            if t.w is not None:
                deps.append(t.w)
            deps.extend(t.r)
        deps += self.fence_pending[q]
        self.fence_pending[q] = []
        fd = []
        seen = set()
        for d in deps:
            if d is o or id(d) in seen:
                continue
            seen.add(id(d))
            if (not d.isdma) and d.q == 'pe' and q == 'pe':
                continue
            d.need = True
            fd.append(d)
        o.deps = fd
        for t in reads:
            t.r.append(o)
        for t in writes:
            t.w = o
            t.r = []
        self.ops[q].append(o)
        return o

    def op(self, q, fn, reads=(), writes=()):
        return self._mk(q, fn, list(reads), list(writes))

    def dma(self, q, fn, semtok, n, reads=(), writes=()):
        o = self._mk(q, fn, list(reads), list(writes))
        o.isdma = True
        o.ndma = n
        key = id(semtok)
        if key not in self.dma_sems:
            self.nsem += 1
            self.dma_sems[key] = [self.stack.enter_context(self.nc.semaphore('ds%d' % self.nsem)), 0]
        ent = self.dma_sems[key]
        ent[1] += 16 * n
        o.sem = ent[0]
        o.val = ent[1]
        o.need = True
        self.dma_since_fence.append(o)
        return o

    def cc(self, fn, reads=(), writes=()):
        o = self._mk('pool', None, list(reads), list(writes))
        o.isdma = True
        self.nsem += 1
        sem = self.stack.enter_context(self.nc.semaphore('cs%d' % self.nsem))
        o.sem = sem
        o.val = 1
        o.need = True
        o.fn = lambda e, s, fn=fn: fn(e).then_inc(s, 1)
        self.dma_since_fence.append(o)
        return o

    def drain(self, q='sp'):
        o = self._mk(q, None, [], [])
        o.deps = list(self.dma_since_fence) + [self.ops[k][-1] for k in QUEUES if self.ops[k] and not self.ops[k][-1].isdma and k != q]
        for d in o.deps:
            d.need = True
        return o

    def fence(self):
        lasts = [self.ops[q][-1] for q in QUEUES if self.ops[q]]
        lasts = [o for o in lasts if not o.isdma]
        allp = lasts + self.dma_since_fence
        for o in allp:
            o.need = True
        for q in QUEUES:
            self.fence_pending[q] = list(allp)
        self.dma_since_fence = []

    def emit(self):
        nc = self.nc
        for q in QUEUES:
            c = 0
            for o in self.ops[q]:
                if o.isdma:
                    continue
                if o.need:
                    c += 1
                    o.sem = self.qsem[q]
                    o.val = c
        self.stats = {q: [len(self.ops[q]), 0] for q in QUEUES}

        def run(q, eng):
            seen = {}
            for o in self.ops[q]:
                for d in o.deps:
                    k = id(d.sem)
                    if seen.get(k, 0) >= d.val:
                        continue
                    seen[k] = d.val
                    eng.wait_ge(d.sem, d.val)
                    self.stats[q][1] += 1
                if o.fn is None:
                    continue
                if o.isdma:
                    o.fn(eng, o.sem)
                else:
                    ins = o.fn(eng)
                    if o.need:
                        ins.then_inc(o.sem, 1)

        with nc.Block() as block:
            @block.tensor
            def _(e):
                run('pe', e)

            @block.scalar
            def _(e):
                run('act', e)

            @block.vector
            def _(e):
                run('dve', e)

            @block.gpsimd
            def _(e):
                run('pool', e)

            @block.sync
            def _(e):
                run('sp', e)


D = 1024
DFF = 2816
NF = DFF // 128
EPS = 1e-6
CH = 512
PANEL_ELEMS = 22 * 256


class Banks:
    def __init__(self, p, n=8, prefix='bk'):
        self.t = [p.ps('%s%d' % (prefix, i), [128, 512], F32) for i in range(n)]
        self.k = [Tok('%s%d' % (prefix, i), excl=True) for i in range(n)]
        self.i = 0
        self.n = n

    def next(self):
        i = self.i
        self.i = (i + 1) % self.n
        return self.t[i], self.k[i]


class Panels:
    def __init__(self, p, n=3):
        self.p = p
        self.b = [p.sb('pan%d' % i, [128, PANEL_ELEMS], BF16) for i in range(n)]
        self.k = [Tok('pan%d' % i) for i in range(n)]
        self.i = 0
        self.n = n

    def load(self, wsrc, nk, c0, ncols, q='pool'):
        i = self.i
        self.i = (i + 1) % self.n
        buf, tok = self.b[i], self.k[i]
        dst = buf[:, 0:nk * ncols].rearrange("p (k c) -> p k c", k=nk)
        srcs, rtoks = wsrc(dst, c0, ncols)

        def fn(e, s, srcs=srcs):
            for d_, s_ in srcs:
                e.dma_start(out=d_, in_=s_).then_inc(s, 16)
        self.p.dma(q, fn, tok, len(srcs), reads=rtoks, writes=[tok])
        return dst, tok


def mm_acc(p, out_ap, pairs, reads, writes):
    pairs = list(pairs)

    def fn(e):
        n = len(pairs)
        ins = None
        for i, (l, r) in enumerate(pairs):
            ins = e.matmul(out_ap, lhsT=l, rhs=r, start=(i == 0), stop=(i == n - 1))
        return ins
    return p.op('pe', fn, reads, writes)


def rstd_from_sq(p, banks, C, sq, t_sq, nk, rstd, t_rstd, tmp, t_tmp, scale, bias_ap, t_bias, W=CH):
    bk, tb = banks.next()
    mm_acc(p, bk[:, 0:W], [(C['ones'][:], sq[:, k, :]) for k in range(nk)], [t_sq, C['t']], [tb])
    p.op('act', lambda e: e.activation(out=tmp, in_=bk[:, 0:W], func=AF.Sqrt, bias=bias_ap, scale=scale),
         [tb, t_bias], [t_tmp])
    p.op('dve', lambda e: e.reciprocal(out=rstd, in_=tmp), [t_tmp], [t_rstd])


def phase2(p, nc, dr, C, src, nchunks):
    banks = Banks(p)
    pans = Panels(p)
    xt = p.sb('p2_xt', [128, 8, CH], F32); t_xt = [Tok('xt%d' % k) for k in range(8)]
    sq = p.sb('p2_sq', [128, 8, CH], BF16); t_sq = Tok('sq')
    hn = p.sb('p2_hn', [128, 8, CH], BF16); t_hn = Tok('hn')
    ya = p.sb('p2_ya', [128, 8, CH], BF16); t_ya = Tok('ya')
    yd = p.sb('p2_yd', [128, 8, CH], BF16); t_yd = Tok('yd')
    sg = p.sb('p2_sg', [128, 16, CH], BF16); t_sg = [Tok('sg%d' % m) for m in range(16)]
    mg = p.sb('p2_mg', [128, 8, CH], BF16); t_mg = [Tok('mg%d' % m) for m in range(8)]
    rr = p.sb('p2_rr', [128, 8, CH], F32); t_rr = [Tok('rr%d' % m) for m in range(8)]
    act = p.sb('p2_act', [128, NF, CH], BF16); t_act = [Tok('act%d' % f) for f in range(NF)]
    rstd = p.sb('p2_rstd', [128, CH], F32); t_rstd = Tok('rstd')
    tmpA = p.sb('p2_tmpA', [128, CH], F32); t_tmpA = Tok('tmpA')
    t1 = [p.sb('p2_t1_%d' % i, [128, CH], F32) for i in range(2)]; t_t1 = [Tok(), Tok()]
    t2 = [p.sb('p2_t2_%d' % i, [128, CH], F32) for i in range(2)]; t_t2 = [Tok(), Tok()]
    sgt = [p.sb('p2_sgt_%d' % i, [128, CH], BF16) for i in range(2)]; t_sgt = [Tok(), Tok()]
    nrm = p.sb('p2_nrm', [128, 32], F32); t_nrm = Tok('nrm')
    epsb = p.sb('p2_eps', [128, 1], F32); t_eps = Tok('eps')
    p.op('pool', lambda e: e.memset(epsb[:], EPS), [], [t_eps])
    p.dma('sp', lambda e, s: e.dma_start(out=nrm[:], in_=dr['nrm2']).then_inc(s, 16), t_nrm, 1, writes=[t_nrm])
    stores = []

    def rmsnorm_fm(wcol0):
        p.op('act', lambda e: e.activation(out=sq[:], in_=xt[:], func=AF.Square), t_xt, [t_sq])
        rstd_from_sq(p, banks, C, sq, t_sq, 8, rstd[:], t_rstd, tmpA[:], t_tmpA, 1.0 / D, epsb[:], t_eps)
        for k in range(8):
            p.op('dve', lambda e, k=k: e.scalar_tensor_tensor(out=hn[:, k, :], in0=xt[:, k, :],
                                                                scalar=nrm[:, wcol0 + k:wcol0 + k + 1],
                                                                in1=rstd[:], op0=ALU.mult, op1=ALU.mult),
                 [t_xt[k], t_rstd, t_nrm], [t_hn])

    def resid_norm(wcol0):
        p.op('act', lambda e: e.activation(out=sq[:], in_=rr[:], func=AF.Square), t_rr, [t_sq])
        rstd_from_sq(p, banks, C, sq, t_sq, 8, rstd[:], t_rstd, tmpA[:], t_tmpA, 1.0 / D, epsb[:], t_eps)
        for m in range(8):
            i = m % 2
            p.op('dve', lambda e, m=m, i=i: e.scalar_tensor_tensor(out=t1[i][:], in0=rr[:, m, :],
                                                                    scalar=nrm[:, wcol0 + m:wcol0 + m + 1],
                                                                    in1=rstd[:], op0=ALU.mult, op1=ALU.mult),
                 [t_rr[m], t_rstd, t_nrm], [t_t1[i]])
            p.op('pool', lambda e, m=m, i=i: e.tensor_tensor(out=xt[:, m, :], in0=xt[:, m, :], in1=t1[i][:], op=ALU.add),
                 [t_t1[i], t_xt[m]], [t_xt[m]])

    for c in range(nchunks):
        t0 = c * CH
        p.dma('sp', lambda e, s, c=c: e.dma_start(out=xt[:], in_=src.x2_chunk(c)).then_inc(s, 16),
              t_xt[0], 1, writes=t_xt)
        src.load_y(p, c, ya, t_ya, yd, t_yd)
        rmsnorm_fm(0)
        for g in range(4):
            pan, tp = pans.load(src.w('w_g'), 8, g * 512, 512)
            for mt in range(4):
                m = g * 4 + mt
                bk, tb = banks.next()
                mm_acc(p, bk[:], [(pan[:, k, mt * 128:(mt + 1) * 128], hn[:, k, :]) for k in range(8)], [tp, t_hn], [tb])
                p.op('act', lambda e, bk=bk, m=m: e.activation(out=sg[:, m, :], in_=bk[:], func=AF.Sigmoid), [tb], [t_sg[m]])
        for g in range(2):
            pa, tpa = pans.load(src.w('w_oa'), 8, g * 512, 512)
            pd, tpd = pans.load(src.w('w_od'), 8, g * 512, 512)
            for mt in range(4):
                m = g * 4 + mt
                i = m % 2
                bA, tA = banks.next()
                mm_acc(p, bA[:], [(pa[:, k, mt * 128:(mt + 1) * 128], ya[:, k, :]) for k in range(8)], [tpa, t_ya], [tA])
                bD, tD = banks.next()
                mm_acc(p, bD[:], [(pd[:, k, mt * 128:(mt + 1) * 128], yd[:, k, :]) for k in range(8)], [tpd, t_yd], [tD])
                p.op('dve', lambda e, bA=bA, m=m, i=i: e.tensor_tensor(out=t1[i][:], in0=bA[:], in1=sg[:, m, :], op=ALU.mult),
                     [tA, t_sg[m]], [t_t1[i]])
                p.op('dve', lambda e, bD=bD, m=m, i=i: e.tensor_tensor(out=t2[i][:], in0=bD[:], in1=sg[:, 8 + m, :], op=ALU.mult),
                     [tD, t_sg[8 + m]], [t_t2[i]])
                p.op('pool', lambda e, m=m, i=i: e.tensor_tensor(out=mg[:, m, :], in0=t1[i][:], in1=t2[i][:], op=ALU.add),
                     [t_t1[i], t_t2[i]], [t_mg[m]])
        for g in range(2):
            pw, tpw = pans.load(src.w('w_out'), 8, g * 512, 512)
            for mt in range(4):
                m = g * 4 + mt
                bk, tb = banks.next()
                mm_acc(p, bk[:], [(pw[:, k, mt * 128:(mt + 1) * 128], mg[:, k, :]) for k in range(8)], [tpw] + t_mg, [tb])
                p.op('act', lambda e, bk=bk, m=m: e.activation(out=rr[:, m, :], in_=bk[:], func=AF.Copy), [tb], [t_rr[m]])
        resid_norm(8)
        rmsnorm_fm(16)
        for g in range(6):
            ncols = 512 if g < 5 else 256
            pg, tpg = pans.load(src.w('w_gate'), 8, g * 512, ncols)
            pu, tpu = pans.load(src.w('w_up'), 8, g * 512, ncols)
            for ft in range(ncols // 128):
                f = g * 4 + ft
                i = f % 2
                bG, tG = banks.next()
                mm_acc(p, bG[:], [(pg[:, k, ft * 128:(ft + 1) * 128], hn[:, k, :]) for k in range(8)], [tpg, t_hn], [tG])
                bU, tU = banks.next()
                mm_acc(p, bU[:], [(pu[:, k, ft * 128:(ft + 1) * 128], hn[:, k, :]) for k in range(8)], [tpu, t_hn], [tU])
                p.op('act', lambda e, bG=bG, i=i: e.activation(out=sgt[i][:], in_=bG[:], func=AF.Silu), [tG], [t_sgt[i]])
                p.op('dve', lambda e, bU=bU, f=f, i=i: e.tensor_tensor(out=act[:, f, :], in0=bU[:], in1=sgt[i][:], op=ALU.mult),
                     [tU, t_sgt[i]], [t_act[f]])
        for g in range(4):
            pw, tpw = pans.load(src.w('w_down'), NF, g * 256, 256)
            for mt in range(2):
                m = g * 2 + mt
                bk, tb = banks.next()
                mm_acc(p, bk[:], [(pw[:, f, mt * 128:(mt + 1) * 128], act[:, f, :]) for f in range(NF)], [tpw] + t_act, [tb])
                p.op('act', lambda e, bk=bk, m=m: e.activation(out=rr[:, m, :], in_=bk[:], func=AF.Copy), [tb], [t_rr[m]])
        resid_norm(24)
        tok_o = Tok('ostore%d' % c)
        so = p.dma('sp', lambda e, s, c=c: e.dma_start(out=src.out_chunk(c), in_=xt[:]).then_inc(s, 16),
                   tok_o, 1, reads=t_xt)
        stores.append(so)
    return stores


NC1 = 962
O_QA, O_KA, O_QD, O_KD, O_VD, O_ZD, O_QAS, O_KAS, O_VA, O_BA = 0, 128, 256, 384, 512, 640, 768, 800, 832, 960
BIG = 30000.0
SCALE = 1.0 / (128.0 ** 0.5)


class Regions:
    def __init__(self, p, nb, dt, prefix):
        per = 4 if dt == F32 else 8
        self.r = []
        for b in range(nb):
            t = p.ps('%s%d' % (prefix, b), [128, 128 * per], dt)
            for i in range(per):
                self.r.append((t[:, i * 128:(i + 1) * 128], Tok('%s%d_%d' % (prefix, b, i))))
        self.i = 0

    def next(self):
        r = self.r[self.i]
        self.i = (self.i + 1) % len(self.r)
        return r


class Rot:
    def __init__(self, p, name, shape, dt, n):
        self.t = [p.sb('%s%d' % (name, i), shape, dt) for i in range(n)]
        self.k = [Tok('%s%d' % (name, i)) for i in range(n)]
        self.i = 0

    def next(self):
        i = self.i
        self.i = (i + 1) % len(self.t)
        return self.t[i], self.k[i]


def phase1(p, nc, dr, C, src, S, nchunks, dbg=None):
    NCHT = S // CH
    banks = Banks(p, 2, 'bkA')
    sbanks = Banks(p, 3, 'bkS')
    bkO = p.ps('bkO', [128, 512], F32); t_bkO = Tok('bkO', excl=True)
    bkD = p.ps('bkD', [128, 512], F32); t_bkD = Tok('bkD', excl=True)
    bkT = p.ps('bkT', [128, 8, 128], BF16); t_bkT = Tok('bkT', excl=True)
    ident = C['identb']; ident32 = C['ident32']; ones = C['ones']; ones32 = C['ones32']
    tC = C['t']

    W1b = p.sb('W1b', [128, 8, NC1], BF16); t_W1 = Tok('W1')
    p.dma('pool', lambda e, s: e.dma_start(out=W1b[:], in_=dr['W1'].rearrange("(k p) c -> p k c", p=128)).then_inc(s, 16),
          t_W1, 1, writes=[t_W1])
    kaT = p.sb('kaT', [128, S], BF16); t_kaT = [Tok('kaT%d' % i) for i in range(NCHT)]
    vaS = p.sb('vaS', [128, S // 128, 128], BF16); t_vaS = [Tok('vaS%d' % i) for i in range(NCHT)]
    kmT = p.sb('kmT', [128, 64], F32); t_kmT = Tok('kmT')
    p.op('pool', lambda e: e.memset(kmT[:], 0.0), [], [t_kmT])
    cau = p.sb('cau', [128, 4, CH], BF16)
    selc = p.sb('selc', [128, 128 + 128 + 127], F32)
    dmask = p.sb('dmask', [128, 7, 128], BF16)
    dnm = p.sb('dnm', [128, 9, 128], F32)
    sm = p.sb('sm', [128, 24], F32); t_sm = Tok('sm')
    t_cst = Tok('cst1')

    def ld_c(e, s):
        e.dma_start(out=cau[:], in_=dr['cau']).then_inc(s, 16)
        e.dma_start(out=selc[:], in_=dr['selc']).then_inc(s, 16)
        e.dma_start(out=dnm[:], in_=dr['dnm']).then_inc(s, 16)
        e.dma_start(out=dmask[:], in_=dr['dmask']).then_inc(s, 16)
        e.dma_start(out=sm[:], in_=dr['sm']).then_inc(s, 16)
    p.dma('sp', ld_c, t_cst, 5, writes=[t_cst, t_sm])
    pastrow = selc[:, 0:128]; negfut = selc[:, 128:256]; ownadj = selc[:, 256:383]
    nmask4 = dnm[:, 0:4, :]; strict4 = dnm[:, 4:8, :]; tri = dnm[:, 8, :]
    epsb = p.sb('p1_eps', [128, 2], F32); t_eps = Tok('eps1')
    p.op('pool', lambda e: e.memset(epsb[:, 0:1], EPS), [], [t_eps])
    p.op('pool', lambda e: e.memset(epsb[:, 1:2], 128.0 * EPS), [t_eps], [t_eps])
    negA = p.sb('negA', [128, 1], F32); t_negA = Tok('negA')
    p.op('act', lambda e: e.activation(out=negA[:], in_=sm[:, 20:21], func=AF.Exp), [t_sm], [t_negA])
    p.op('pool', lambda e: e.tensor_scalar(out=negA[:], in0=negA[:], scalar1=-1.0, scalar2=0.0, op0=ALU.mult, op1=ALU.add),
         [t_negA], [t_negA])

    xt = p.sb('p1_xt', [128, 8, CH], F32); t_xt = Tok('xt1')
    sq = p.sb('p1_sq', [128, 8, CH], BF16); t_sq = Tok('sq1')
    hn = p.sb('p1_hn', [128, 8, CH], BF16); t_hn = Tok('hn1')
    rstd = p.sb('p1_rstd', [128, CH], F32); t_rstd = Tok('rstd1')
    tmpA = p.sb('p1_tmpA', [128, CH], F32); t_tmpA = Tok('tmpA1')
    ropeC = p.sb('ropeC', [32, CH], F32); ropeS = p.sb('ropeS', [32, CH], F32); t_rope = Tok('rope')
    q32 = p.sb('q32', [128, CH], F32); t_q32 = Tok('q32')
    k32 = p.sb('k32', [128, CH], F32); t_k32 = Tok('k32')
    rtmp = Rot(p, 'rtmp', [32, CH], F32, 2)
    qTb = p.sb('qTb', [128, CH], BF16); t_qTb = Tok('qTb')
    raw = {n: p.sb('raw%s' % n, [128, CH + 3], F32) for n in 'qkv'}
    t_raw = {n: Tok('raw%s' % n) for n in 'qkv'}
    for n in 'qkv':
        p.op('pool', lambda e, n=n: e.memset(raw[n][:, 0:3], 0.0), [], [t_raw[n]])
    cacc = Rot(p, 'cacc', [128, CH], F32, 1)
    sil = Rot(p, 'sil', [128, CH], F32, 1)
    sqd = p.sb('sqd', [128, 1, CH], BF16); t_sqd = Tok('sqd')
    rn = rstd; t_rn = t_rstd
    tmpB = tmpA; t_tmpB = t_tmpA
    qdT = p.sb('qdT', [128, CH], BF16); t_qdT = Tok('qdT')
    kdT = p.sb('kdT', [128, CH], BF16); t_kdT = Tok('kdT')
    vdT = p.sb('vdT', [128, CH], BF16); t_vdT = Tok('vdT')
    szT = p.sb('szT', [128, CH], BF16); t_szT = Tok('szT')
    ba = p.sb('ba', [128, 4, 2], F32); t_ba = Tok('ba')
    gsel = p.sb('gsel', [128, 64], F32); t_gsel = Tok('gsel')
    m8 = p.sb('m8', [128, 8], F32); t_m8 = Tok('m8')
    sel2 = p.sb('sel2', [128, 64], F32); t_sel2 = Tok('sel2')
    negm = p.sb('negm', [128, 4, 64], F32); t_negm = Tok('negm')
    negmT = p.sb('negmT', [64, CH], BF16); t_negmT = Tok('negmT')
    pT = Rot(p, 'pT', [128, CH], BF16, 3)
    rden = p.sb('rden', [128, CH], F32); t_rden = Tok('rden')
    yaO = Rot(p, 'yaO', [128, CH], BF16, 1)
    ydO = Rot(p, 'ydO', [128, CH], BF16, 1)
    sc = p.sb('dsc', [128, 12, 4], F32)
    t_sc = [Tok('dsc%d' % i) for i in range(12)]
    (I_BETA, I_E1, I_SP, I_G, I_GC, I_GL, I_EG, I_EGL, I_KDS, I_BGE, I_NB, I_T) = range(12)
    Sst32 = p.sb('S32', [128, 128], F32); t_S32 = Tok('S32')
    Sbf = p.sb('Sbf', [128, 128], BF16); t_Sbf = Tok('Sbf')
    p.op('pool', lambda e: e.memset(Sst32[:], 0.0), [], [t_S32])
    p.op('pool', lambda e: e.memset(Sbf[:], 0.0), [], [t_Sbf])
    osb = p.sb('osb', [128, CH], F32); t_osb = [Tok('osb%d' % i) for i in range(4)]
    NS = 4
    f32n = ['diagG', 'arg', 'Dincl']
    bfn = ['egbc', 'attn', 'attnT', 'qgT', 'kbg', 'kdec', 'vb', 'negWT', 'vnew',
           'X0', 'XL0', 'PL0', 'PL1', 'PU0', 'PU1', 'R0', 'R1', 'RU0', 'RU1', 'NoL', 'NoU']
    dtl = {n: p.sb('dn_%s' % n, [128, NS, 128], F32) for n in f32n}
    dtl.update({n: p.sb('dn_%s' % n, [128, NS, 128], BF16) for n in bfn})
    dtl['kvtok'] = p.sb('dn_kvtok', [128, 8, 128], BF16)
    dk = {n: Tok('dn_%s' % n) for n in f32n + bfn + ['kvtok']}
    dtl['Dstr'] = dtl['diagG']; dk['Dstr'] = dk['diagG']
    dkv = {n: [Tok('dn_%s%d' % (n, i)) for i in range(NS)] for n in ['vnew']}

    DNSTEPS = [44]
    stores = []

    def act_copy(out, in_, reads, writes, q='act'):
        if q == 'act':
            return p.op('act', lambda e: e.activation(out=out, in_=in_, func=AF.Copy), reads, writes)
        return p.op(q, lambda e: e.tensor_copy(out=out, in_=in_), reads, writes)

    for t in range(nchunks):
        t0 = t * CH
        b = t % 2
        p.dma('sp', lambda e, s, t=t: e.dma_start(out=xt[:], in_=src.x_chunk(t)).then_inc(s, 16),
              t_xt, 1, reads=src.x_toks(t), writes=[t_xt])

        def ld_rope(e, s, t=t):
            e.dma_start(out=ropeC[:], in_=src.rope(t, 0)).then_inc(s, 16)
            e.dma_start(out=ropeS[:], in_=src.rope(t, 1)).then_inc(s, 16)
        p.dma('sp', ld_rope, t_rope, 2, reads=src.rope_toks(t), writes=[t_rope])
        p.op('act', lambda e: e.activation(out=sq[:], in_=xt[:], func=AF.Square), [t_xt], [t_sq])
        rstd_from_sq(p, banks, C, sq, t_sq, 8, rstd[:], t_rstd, tmpA[:], t_tmpA, 1.0 / D, epsb[:, 0:1], t_eps)
        for k in range(8):
            p.op('dve', lambda e, k=k: e.scalar_tensor_tensor(out=hn[:, k, :], in0=xt[:, k, :], scalar=sm[:, k:k + 1],
                                                                in1=rstd[:], op0=ALU.mult, op1=ALU.mult),
                 [t_xt, t_rstd, t_sm], [t_hn])

        def proj(col0, ncol):
            bk, tb = banks.next()
            mm_acc(p, bk[0:ncol, :], [(W1b[:, k, col0:col0 + ncol], hn[:, k, :]) for k in range(8)], [t_W1, t_hn], [tb])
            return bk, tb

        for (oc, ocs, dst32, t_dst, isq) in ((O_QA, O_QAS, q32, t_q32, True), (O_KA, O_KAS, k32, t_k32, False)):
            bk, tb = proj(oc, 128)
            bs, tbs = proj(ocs, 32)
            act_copy(dst32[:], bk[:], [tb], [t_dst])
            rt, t_rt = rtmp.next()
            p.op('dve', lambda e, bs=bs, rt=rt: e.tensor_tensor(out=rt[:], in0=bs[0:32, :], in1=ropeS[:], op=ALU.mult),
                 [tbs, t_rope], [t_rt])
            p.op('pool', lambda e, d=dst32: e.tensor_tensor(out=d[0:32, :], in0=d[0:32, :], in1=ropeC[:], op=ALU.mult),
                 [t_dst, t_rope], [t_dst])
            p.op('pool', lambda e, d=dst32, rt=rt: e.tensor_tensor(out=d[0:32, :], in0=d[0:32, :], in1=rt[:], op=ALU.add),
                 [t_dst, t_rt], [t_dst])
            if isq:
                act_copy(qTb[:], q32[:], [t_q32], [t_qTb], q='pool')
            else:
                act_copy(kaT[:, t0:t0 + CH], k32[:], [t_k32], [t_kaT[t]], q='pool')
                p.op('dve', lambda e, t=t: e.tensor_reduce(out=kmT[:, 2 * t:2 * t + 2],
                                                             in_=k32[:].rearrange("p (b t) -> p b t", b=2),
                                                             axis=AX.X, op=ALU.add), [t_k32], [t_kmT])
        for i, (n, oc) in enumerate((('q', O_QD), ('k', O_KD), ('v', O_VD))):
            bk, tb = proj(oc, 128)
            rw = raw[n]; t_rw = t_raw[n]
            if t > 0:
                p.op('pool', lambda e, rw=rw: e.tensor_copy(out=rw[:, 0:3], in_=rw[:, CH:CH + 3]), [t_rw], [t_rw])
            act_copy(rw[:, 3:CH + 3], bk[:], [tb], [t_rw])
            ca, t_ca = cacc.next()
            p.op('dve', lambda e, rw=rw, ca=ca, i=i: e.tensor_scalar(out=ca[:], in0=rw[:, 0:CH], scalar1=sm[:, 8 + 4 * i:9 + 4 * i],
                                                                      scalar2=None, op0=ALU.mult), [t_rw, t_sm], [t_ca])
            for j in range(1, 4):
                p.op('dve', lambda e, rw=rw, ca=ca, i=i, j=j: e.scalar_tensor_tensor(
                    out=ca[:], in0=rw[:, j:j + CH], scalar=sm[:, 8 + 4 * i + j:9 + 4 * i + j], in1=ca[:],
                    op0=ALU.mult, op1=ALU.add), [t_rw, t_sm, t_ca], [t_ca])
            if n == 'v':
                p.op('act', lambda e, ca=ca: e.activation(out=vdT[:], in_=ca[:], func=AF.Silu), [t_ca], [t_vdT])
            else:
                so, t_so = sil.next()
                p.op('act', lambda e, ca=ca, so=so: e.activation(out=so[:], in_=ca[:], func=AF.Silu), [t_ca], [t_so])
                p.op('pool', lambda e, so=so: e.tensor_tensor(out=sqd[:, 0, :], in0=so[:], in1=so[:], op=ALU.mult),
                     [t_so], [t_sqd])
                if n == 'q':
                    rstd_from_sq(p, banks, C, sqd, t_sqd, 1, rn[:], t_rn, tmpB[:], t_tmpB, 128.0, epsb[:, 1:2], t_eps)
                    p.op('dve', lambda e, so=so: e.tensor_tensor(out=qdT[:], in0=so[:], in1=rn[:], op=ALU.mult),
                         [t_so, t_rn], [t_qdT])
                else:
                    rstd_from_sq(p, banks, C, sqd, t_sqd, 1, rn[:], t_rn, tmpB[:], t_tmpB, 1.0, epsb[:, 0:1], t_eps)
                    p.op('dve', lambda e, so=so: e.tensor_tensor(out=kdT[:], in0=so[:], in1=rn[:], op=ALU.mult),
                         [t_so, t_rn], [t_kdT])
        bk, tb = proj(O_ZD, 128)
        p.op('act', lambda e, bk=bk: e.activation(out=szT[:], in_=bk[:], func=AF.Silu), [tb], [t_szT])
        for tt in range(4):
            bk, tb = banks.next()
            mm_acc(p, bk[:, 0:130], [(hn[:, k, tt * 128:(tt + 1) * 128], W1b[:, k, O_VA:O_VA + 130]) for k in range(8)],
                   [t_W1, t_hn], [tb])
            act_copy(vaS[:, 4 * t + tt, :], bk[:, 0:128], [tb], [t_vaS[t]])
            p.op('dve', lambda e, bk=bk, tt=tt: e.tensor_copy(out=ba[:, tt, :], in_=bk[:, 128:130]), [tb], [t_ba])
        bG, t_bG = banks.next()

        def gates(e, bG=bG):
            ins = None
            for tt in range(4):
                ins = e.matmul(bG[:, tt * 64:(tt + 1) * 64], lhsT=q32[:, tt * 128:(tt + 1) * 128], rhs=kmT[:],
                               start=True, stop=True)
            return ins
        p.op('pe', gates, [t_q32, t_kmT], [t_bG])
        for tt in range(4):
            blk = 2 * t + tt // 2
            p.op('dve', lambda e, bG=bG, blk=blk, tt=tt: e.tensor_tensor(out=gsel[:], in0=bG[:, tt * 64:(tt + 1) * 64],
                                                                          in1=negfut[:, 64 - blk:128 - blk], op=ALU.add),
                 [t_bG, t_cst], [t_gsel])
            p.op('dve', lambda e: e.max(out=m8[:], in_=gsel[:]), [t_gsel], [t_m8])
            p.op('dve', lambda e, blk=blk: e.scalar_tensor_tensor(out=sel2[:], in0=gsel[:], scalar=m8[:, 2:3],
                                                                   in1=pastrow[:, 64 - blk:128 - blk],
                                                                   op0=ALU.is_ge, op1=ALU.mult),
                 [t_gsel, t_m8, t_cst], [t_sel2])
            p.op('dve', lambda e, blk=blk, tt=tt: e.scalar_tensor_tensor(out=negm[:, tt, :], in0=sel2[:], scalar=BIG,
                                                                          in1=ownadj[:, 63 - blk:127 - blk],
                                                                          op0=ALU.mult, op1=ALU.add),
                 [t_sel2, t_cst], [t_negm])
        bG2, t_bG2 = banks.next()

        def ntr(e, bG2=bG2):
            ins = None
            for tt in range(4):
                ins = e.transpose(bG2[0:64, tt * 128:(tt + 1) * 128], negm[:, tt, :], ident32[:])
            return ins
        p.op('pe', ntr, [t_negm, tC], [t_bG2])
        act_copy(negmT[:], bG2[0:64, :], [t_bG2], [t_negmT])
        if dbg is not None and t < dbg['n']:
            td = Tok()
            def dd(e, s, t0=t0):
                e.dma_start(out=dr['dbg_q32'][:, t0:t0 + CH], in_=q32[:]).then_inc(s, 16)
                e.dma_start(out=dr['dbg_k32'][:, t0:t0 + CH], in_=k32[:]).then_inc(s, 16)
                e.dma_start(out=dr['dbg_negmT'][:, t0:t0 + CH], in_=negmT[:]).then_inc(s, 16)
                e.dma_start(out=dr['dbg_qdT'][:, t0:t0 + CH], in_=qdT[:]).then_inc(s, 16)
                e.dma_start(out=dr['dbg_kdT'][:, t0:t0 + CH], in_=kdT[:]).then_inc(s, 16)
                e.dma_start(out=dr['dbg_vdT'][:, t0:t0 + CH], in_=vdT[:]).then_inc(s, 16)
            stores.append(p.dma('sp', dd, td, 6, reads=[t_q32, t_k32, t_negmT, t_qdT, t_kdT, t_vdT]))
        def moba_gen(t=t, t0=t0):
            nkt = 4 * (t + 1)
            LOOK = 2
            pend = []

            def emit_pv(kt, pt, t_pt, nkt=nkt):
                def pv(e, kt=kt, pt=pt, nkt=nkt):
                    e.matmul(bkO[:], lhsT=vaS[:, kt, :], rhs=pt[:], start=(kt == 0), stop=(kt == nkt - 1))
                    return e.matmul(bkD[:], lhsT=ones[:], rhs=pt[:], start=(kt == 0), stop=(kt == nkt - 1))
                p.op('pe', pv, [t_vaS[kt // 4], t_pt, tC], [t_bkO, t_bkD])
            for kt in range(nkt):
                j = kt // 2
                bk, tb = sbanks.next()
                prs = [(kaT[:, kt * 128:(kt + 1) * 128], qTb[:]), (ident[0:64, j:j + 1].broadcast_to([64, 128]), negmT[:])]
                if kt >= 4 * t:
                    prs.append((ident[:], cau[:, kt - 4 * t, :]))
                mm_acc(p, bk[:], prs, [t_kaT[kt // 4], t_qTb, t_negmT, t_cst, tC], [tb])
                pt, t_pt = pT.next()
                p.op('act', lambda e, bk=bk, pt=pt: e.activation(out=pt[:], in_=bk[:], func=AF.Exp, scale=SCALE), [tb], [t_pt])
                pend.append((kt, pt, t_pt))
                if len(pend) > LOOK:
                    emit_pv(*pend.pop(0))
                yield
            while pend:
                emit_pv(*pend.pop(0))
            p.op('dve', lambda e: e.reciprocal(out=rden[:], in_=bkD[:]), [t_bkD], [t_rden])
            yo, t_yo = yaO.next()
            p.op('dve', lambda e, yo=yo: e.tensor_tensor(out=yo[:], in0=bkO[:], in1=rden[:], op=ALU.mult), [t_bkO, t_rden], [t_yo])
            src.store_y(p, t, 0, yo, t_yo)

            yield

        def dn_gen(t=t, t0=t0):
            def scal(i):
                return sc[:, i, :]
            bav = ba[:].rearrange("p n c -> p c n")
            p.op('act', lambda e: e.activation(out=scal(I_BETA), in_=bav[:, 0, :], func=AF.Sigmoid), [t_ba], [t_sc[I_BETA]])
            p.op('act', lambda e: e.activation(out=scal(I_E1), in_=bav[:, 1, :], func=AF.Exp, bias=sm[:, 21:22]),
                 [t_ba, t_sm], [t_sc[I_E1]])
            p.op('act', lambda e: e.activation(out=scal(I_SP), in_=scal(I_E1), func=AF.Ln, bias=1.0), [t_sc[I_E1]], [t_sc[I_SP]])
            p.op('dve', lambda e: e.tensor_scalar(out=scal(I_G), in0=scal(I_SP), scalar1=negA[:, 0:1], scalar2=None, op0=ALU.mult),
                 [t_sc[I_SP], t_negA], [t_sc[I_G]])
            rg, t_rg = banks.next()

            def gcs(e, rg=rg):
                e.matmul(rg[:, 0:4], lhsT=tri, rhs=scal(I_G), start=True, stop=True)
                return e.matmul(rg[:, 4:8], lhsT=ones32[:], rhs=scal(I_G), start=True, stop=True)
            p.op('pe', gcs, [t_sc[I_G], t_cst, tC], [t_rg])
            p.op('dve', lambda e, rg=rg: e.tensor_copy(out=scal(I_GC), in_=rg[:, 0:4]), [t_rg], [t_sc[I_GC]])
            p.op('dve', lambda e, rg=rg: e.tensor_copy(out=scal(I_GL), in_=rg[:, 4:8]), [t_rg], [t_sc[I_GL]])
            p.op('act', lambda e: e.activation(out=scal(I_EG), in_=scal(I_GC), func=AF.Exp), [t_sc[I_GC]], [t_sc[I_EG]])
            p.op('act', lambda e: e.activation(out=scal(I_EGL), in_=scal(I_GL), func=AF.Exp), [t_sc[I_GL]], [t_sc[I_EGL]])
            p.op('dve', lambda e: e.tensor_tensor(out=scal(I_T), in0=scal(I_GL), in1=scal(I_GC), op=ALU.subtract),
                 [t_sc[I_GL], t_sc[I_GC]], [t_sc[I_T]])
            p.op('act', lambda e: e.activation(out=scal(I_KDS), in_=scal(I_T), func=AF.Exp), [t_sc[I_T]], [t_sc[I_KDS]])
            p.op('dve', lambda e: e.tensor_tensor(out=scal(I_BGE), in0=scal(I_BETA), in1=scal(I_EG), op=ALU.mult),
                 [t_sc[I_BETA], t_sc[I_EG]], [t_sc[I_BGE]])
            p.op('dve', lambda e: e.tensor_scalar(out=scal(I_NB), in0=scal(I_BETA), scalar1=-1.0, scalar2=None, op0=ALU.mult),
                 [t_sc[I_BETA]], [t_sc[I_NB]])

            def col(i, n):
                return sc[:, i, n:n + 1]

            T_ = dtl
            for n in range(NS):
                p.op('pool', lambda e, n=n: e.tensor_scalar(out=T_['diagG'][:, n, :], in0=ident32[:], scalar1=col(I_GC, n),
                                                             scalar2=0.0, op0=ALU.mult, op1=ALU.add), [t_sc[I_GC], tC], [dk['diagG']])
            bBC, t_bBC = banks.next()
            p.op('pe', lambda e, bBC=bBC: e.matmul(bBC[:], lhsT=ones32[:], rhs=T_['diagG'][:].rearrange("p n c -> p (n c)"),
                                                   start=True, stop=True), [dk['diagG'], tC], [t_bBC])
            p.op('dve', lambda e, bBC=bBC: e.scalar_tensor_tensor(out=T_['arg'][:].rearrange("p n c -> p (n c)"), in0=bBC[:], scalar=-1.0,
                                                                  in1=nmask4.rearrange("p n c -> p (n c)"), op0=ALU.mult, op1=ALU.add),
                 [t_bBC, t_cst], [dk['arg']])
            p.op('act', lambda e, bBC=bBC: e.activation(out=T_['egbc'][:].rearrange("p n c -> p (n c)"), in_=bBC[:], func=AF.Exp),
                 [t_bBC], [dk['egbc']])
            for n in range(NS):
                p.op('act', lambda e, n=n: e.activation(out=T_['Dincl'][:, n, :], in_=T_['arg'][:, n, :], func=AF.Exp, bias=col(I_GC, n)),
                     [dk['arg'], t_sc[I_GC]], [dk['Dincl']])
            p.op('pool', lambda e: e.tensor_tensor(out=T_['Dstr'][:], in0=T_['Dincl'][:], in1=strict4, op=ALU.mult),
                 [dk['Dincl'], t_cst], [dk['Dstr']])
            bKK, t_bKK = banks.next()

            def kkf(e, bKK=bKK):
                ins = None
                for n in range(NS):
                    cs = slice(n * 128, (n + 1) * 128)
                    ins = e.matmul(bKK[:, cs], lhsT=kdT[:, cs], rhs=kdT[:, cs], start=True, stop=True)
                return ins
            p.op('pe', kkf, [t_kdT], [t_bKK])
            yield
            bQK, t_bQK = banks.next()

            def qkf(e, bQK=bQK):
                ins = None
                for n in range(NS):
                    cs = slice(n * 128, (n + 1) * 128)
                    ins = e.matmul(bQK[:, cs], lhsT=qdT[:, cs], rhs=kdT[:, cs], start=True, stop=True)
                return ins
            p.op('pe', qkf, [t_kdT, t_qdT], [t_bQK])
            for n in range(NS):
                p.op('dve', lambda e, bKK=bKK, n=n: e.scalar_tensor_tensor(out=T_['XL0'][:, n, :], in0=bKK[:, n * 128:(n + 1) * 128],
                                                                            scalar=col(I_NB, n), in1=T_['Dstr'][:, n, :],
                                                                            op0=ALU.mult, op1=ALU.mult),
                     [t_bKK, t_sc[I_NB], dk['Dstr']], [dk['XL0']])
            p.op('dve', lambda e, bQK=bQK: e.tensor_tensor(out=T_['attn'][:].rearrange("p n c -> p (n c)"), in0=bQK[:],
                                                           in1=T_['Dincl'][:].rearrange("p n c -> p (n c)"), op=ALU.mult),
                 [t_bQK, dk['Dincl']], [dk['attn']])

            def trf(e):
                ins = None
                for n in range(NS):
                    ins = e.transpose(bkT[:, n, :], T_['XL0'][:, n, :], ident[:])
                for n in range(NS):
                    ins = e.transpose(bkT[:, 4 + n, :], T_['attn'][:, n, :], ident[:])
                return ins
            p.op('pe', trf, [dk['XL0'], dk['attn'], tC], [t_bkT])
            yield
            act_copy(T_['X0'][:], bkT[:, 0:4, :], [t_bkT], [dk['X0']])
            p.op('dve', lambda e: e.tensor_copy(out=T_['attnT'][:], in_=bkT[:, 4:8, :]), [t_bkT], [dk['attnT']])
            p.op('pool', lambda e: e.tensor_tensor(out=T_['qgT'][:].rearrange("p n c -> p (n c)"), in0=qdT[:],
                                                   in1=T_['egbc'][:].rearrange("p n c -> p (n c)"), op=ALU.mult),
                 [t_qdT, dk['egbc']], [dk['qgT']])

            def trkv(e):
                ins = None
                for n in range(NS):
                    ins = e.transpose(bkT[:, n, :], kdT[:, n * 128:(n + 1) * 128], ident[:])
                for n in range(NS):
                    ins = e.transpose(bkT[:, 4 + n, :], vdT[:, n * 128:(n + 1) * 128], ident[:])
                return ins
            p.op('pe', trkv, [t_kdT, t_vdT, tC], [t_bkT])
            yield
            act_copy(T_['kvtok'][:], bkT[:], [t_bkT], [dk['kvtok']])
            for n in range(NS):
                p.op('pool', lambda e, n=n: e.tensor_scalar(out=T_['kbg'][:, n, :], in0=T_['kvtok'][:, n, :], scalar1=col(I_BGE, n),
                                                             scalar2=0.0, op0=ALU.mult, op1=ALU.add), [dk['kvtok'], t_sc[I_BGE]], [dk['kbg']])
                p.op('pool', lambda e, n=n: e.tensor_scalar(out=T_['kdec'][:, n, :], in0=T_['kvtok'][:, n, :], scalar1=col(I_KDS, n),
                                                             scalar2=0.0, op0=ALU.mult, op1=ALU.add), [dk['kvtok'], t_sc[I_KDS]], [dk['kdec']])
                p.op('pool', lambda e, n=n: e.tensor_scalar(out=T_['vb'][:, n, :], in0=T_['kvtok'][:, 4 + n, :], scalar1=col(I_BETA, n),
                                                             scalar2=0.0, op0=ALU.mult, op1=ALU.add), [dk['kvtok'], t_sc[I_BETA]], [dk['vb']])
            def msk(dst, src, mi):
                p.op('pool', lambda e: e.tensor_tensor(out=T_[dst][:], in0=T_[src][:],
                                                       in1=dmask[:, mi:mi + 1, :].broadcast_to([128, NS, 128]), op=ALU.mult),
                     [dk[src], t_cst], [dk[dst]])

            def addI(dst, src):
                p.op('pool', lambda e: e.tensor_tensor(out=T_[dst][:], in0=T_[src][:],
                                                       in1=ident[:].unsqueeze(1).broadcast_to([128, NS, 128]), op=ALU.add),
                     [dk[src], tC], [dk[dst]])

            def mm4(pairs_of, reads, evac_to, q):
                bk, tb = banks.next()

                def fn(e, bk=bk):
                    ins = None
                    for n in range(NS):
                        o = bk[:, n * 128:(n + 1) * 128]
                        prs = pairs_of(n)
                        for i, (l, r) in enumerate(prs):
                            ins = e.matmul(o, lhsT=l, rhs=r, start=(i == 0), stop=(i == len(prs) - 1))
                    return ins
                p.op('pe', fn, reads, [tb])
                dst = T_[evac_to][:].rearrange("p n c -> p (n c)")
                if q == 'act':
                    act_copy(dst, bk[:], [tb], [dk[evac_to]])
                else:
                    p.op('dve', lambda e, bk=bk: e.tensor_copy(out=dst, in_=bk[:]), [tb], [dk[evac_to]])

            msk('PL0', 'XL0', 0)
            msk('PU0', 'X0', 0)
            addI('R0', 'PL0')
            addI('RU0', 'PU0')
            cur = 0
            for s_ in range(3):
                nx = 1 - cur
                PL, PU, R, RU = 'PL%d' % cur, 'PU%d' % cur, 'R%d' % cur, 'RU%d' % cur
                PLn, PUn, Rn, RUn = 'PL%d' % nx, 'PU%d' % nx, 'R%d' % nx, 'RU%d' % nx
                mm4(lambda n, PL=PL, PU=PU: [(T_[PU][:, n, :], T_[PL][:, n, :])], [dk[PL], dk[PU]], PLn, 'act')
                yield
                mm4(lambda n, PL=PL, PU=PU: [(T_[PL][:, n, :], T_[PU][:, n, :])], [dk[PL], dk[PU]], PUn, 'dve')
                yield
                mm4(lambda n, RU=RU, R=R, PLn=PLn: [(T_[RU][:, n, :], T_[PLn][:, n, :]), (ident[:], T_[R][:, n, :])],
                    [dk[RU], dk[R], dk[PLn], tC], Rn, 'act')
                yield
                mm4(lambda n, RU=RU, R=R, PUn=PUn: [(T_[R][:, n, :], T_[PUn][:, n, :]), (ident[:], T_[RU][:, n, :])],
                    [dk[RU], dk[R], dk[PUn], tC], RUn, 'dve')
                yield
                cur = nx
            for lv in range(1, 4):
                nx = 1 - cur
                Z, Y, Zn, Yn = 'R%d' % cur, 'RU%d' % cur, 'R%d' % nx, 'RU%d' % nx
                msk('NoL', 'XL0', lv)
                msk('NoU', 'X0', 3 + lv)
                if lv < 3:
                    mm4(lambda n, Z=Z: [(T_['NoU'][:, n, :], T_[Z][:, n, :])], [dk['NoU'], dk[Z]], 'PL0', 'act')
                    yield
                mm4(lambda n, Y=Y: [(T_['NoL'][:, n, :], T_[Y][:, n, :])], [dk['NoL'], dk[Y]], 'PU0', 'dve')
                yield
                if lv < 3:
                    mm4(lambda n, Z=Z, Y=Y: [(T_[Y][:, n, :], T_['PL0'][:, n, :]), (ident[:], T_[Z][:, n, :])],
                        [dk[Y], dk[Z], dk['PL0'], tC], Zn, 'act')
                    yield
                mm4(lambda n, Z=Z, Y=Y: [(T_[Z][:, n, :], T_['PU0'][:, n, :]), (ident[:], T_[Y][:, n, :])],
                    [dk[Y], dk[Z], dk['PU0'], tC], Yn, 'dve')
                yield
                cur = nx
            XF = 'RU%d' % cur
            b5, t_b5 = banks.next()

            def f5(e, b5=b5):
                ins = None
                for n in range(NS):
                    ins = e.matmul(b5[:, n * 128:(n + 1) * 128], lhsT=T_['kbg'][:, n, :], rhs=T_[XF][:, n, :], start=True, stop=True)
                return ins
            p.op('pe', f5, [dk['kbg'], dk[XF]], [t_b5])
            yield
            p.op('act', lambda e, b5=b5: e.mul(out=T_['negWT'][:].rearrange("p n c -> p (n c)"), in_=b5[:], mul=-1.0), [t_b5], [dk['negWT']])
            bOT, t_bOT = banks.t[0], banks.k[0]
            for n in range(NS):
                bV, t_bV = banks.t[1], banks.k[1]
                mm_acc(p, bV[:, 0:128], [(T_[XF][:, n, :], T_['vb'][:, n, :]), (T_['negWT'][:, n, :], Sbf[:])],
                       [dk[XF], dk['vb'], dk['negWT'], t_Sbf, tC], [t_bV])
                act_copy(T_['vnew'][:, n, :], bV[:, 0:128], [t_bV], [dkv['vnew'][n]])
                yield
                mm_acc(p, bOT[:, n * 128:(n + 1) * 128], [(Sbf[:], T_['qgT'][:, n, :]), (T_['vnew'][:, n, :], T_['attnT'][:, n, :])],
                       [t_Sbf, dk['qgT'], dkv['vnew'][n], dk['attnT']], [t_bOT])
                bS, t_bS = banks.t[1], banks.k[1]
                p.op('pe', lambda e, bS=bS, n=n: e.matmul(bS[:, 128:256], lhsT=T_['kdec'][:, n, :], rhs=T_['vnew'][:, n, :], start=True, stop=True),
                     [dk['kdec'], dkv['vnew'][n]], [t_bS])
                p.op('dve', lambda e, bS=bS, n=n: e.scalar_tensor_tensor(out=Sbf[:], in0=Sst32[:], scalar=col(I_EGL, n), in1=bS[:, 128:256],
                                                                          op0=ALU.mult, op1=ALU.add),
                     [t_S32, t_sc[I_EGL], t_bS], [t_Sbf])
                p.op('dve', lambda e, bS=bS, n=n: e.scalar_tensor_tensor(out=Sst32[:], in0=Sst32[:], scalar=col(I_EGL, n), in1=bS[:, 128:256],
                                                                          op0=ALU.mult, op1=ALU.add),
                     [t_S32, t_sc[I_EGL], t_bS], [t_S32])
                yield
            act_copy(osb[:], bOT[:], [t_bOT], t_osb)
            yield
            p.op('act', lambda e: e.activation(out=sqd[:, 0, :], in_=osb[:], func=AF.Square), t_osb, [t_sqd])
            rstd_from_sq(p, banks, C, sqd, t_sqd, 1, rn[:], t_rn, tmpB[:], t_tmpB, 1.0 / 128.0, epsb[:, 0:1], t_eps)
            p.op('dve', lambda e: e.scalar_tensor_tensor(out=tmpB[:], in0=osb[:], scalar=sm[:, 22:23], in1=rn[:],
                                                         op0=ALU.mult, op1=ALU.mult), t_osb + [t_rn, t_sm, t_tmpB], [t_tmpB])
            yo2, t_yo2 = ydO.next()
            p.op('pool', lambda e, yo2=yo2: e.tensor_tensor(out=yo2[:], in0=tmpB[:], in1=szT[:], op=ALU.mult),
                 [t_tmpB, t_szT], [t_yo2])
            src.store_y(p, t, 1, yo2, t_yo2)
            yield

        g1, g2 = moba_gen(), dn_gen()
        n1, n2 = 4 * (t + 1) + 1, DNSTEPS[0]
        i1 = i2 = 0
        d1 = d2 = False
        while not (d1 and d2):
            if d1 or (not d2 and i2 * n1 <= i1 * n2):
                try:
                    next(g2); i2 += 1
                except StopIteration:
                    d2 = True
            else:
                try:
                    next(g1); i1 += 1
                except StopIteration:
                    d1 = True
        DNSTEPS[0] = max(i2, 1)
        src.after_chunk(t)
    return stores


IN_SIZES = [1024, 1024, 1024, 1024, 1024, 1024, 1024, 8, 8, 1024, 1024]
OFF = np.concatenate([[0], np.cumsum(IN_SIZES)]).astype(int)
BIG = 30000.0


def nl(v):
    return np.ascontiguousarray(np.asarray(v, np.float32).reshape(8, 128).T)


def make_consts():
    c = {}
    c['identb'] = np.eye(128, dtype=np.float32).astype(bf)
    c['ident32'] = np.eye(128, dtype=np.float32)
    c['ones'] = np.ones((128, 128), bf)
    c['ones32'] = np.ones((128, 128), np.float32)
    Fh = np.zeros((64, 64, 128), np.float32)
    for j in range(64):
        Fh[j, j, :] = 1.0
    c['Fh'] = Fh.reshape(64, 64 * 128).astype(bf)
    kk = np.arange(128)[:, None, None] + 128 * np.arange(4)[None, :, None]
    qq = np.arange(512)[None, None, :]
    c['cau'] = np.where(kk > qq, -BIG, 0.0).astype(np.float32).astype(bf)
    col = np.arange(128)
    pastrow = np.broadcast_to((col < 64).astype(np.float32), (128, 128))
    negfut = np.broadcast_to(np.where(col < 64, 0.0, -1e30).astype(np.float32), (128, 128))
    ownadj = np.broadcast_to(np.where(np.arange(127) == 63, 0.0, -BIG).astype(np.float32), (128, 127))
    c['selc'] = np.ascontiguousarray(np.concatenate([pastrow, negfut, ownadj], axis=1))
    cc = np.arange(128)[:, None]; jj = np.arange(128)[None, :]
    nmask = np.where(jj <= cc, 0.0, -BIG).astype(np.float32)
    strict = (jj < cc).astype(np.float32)
    tri = (cc <= jj).astype(np.float32)
    c['dnm'] = np.ascontiguousarray(np.stack([nmask] * 4 + [strict] * 4 + [tri], axis=1))
    b16 = (cc // 16 == jj // 16)
    mL1 = (cc // 32 == jj // 32) & (cc // 16 > jj // 16)
    mL2 = (cc // 64 == jj // 64) & (cc // 32 > jj // 32)
    mL3 = (cc // 64 > jj // 64)
    c['dmask'] = np.ascontiguousarray(np.stack([b16, mL1, mL2, mL3, mL1.T, mL2.T, mL3.T], axis=1).astype(np.float32)).astype(bf)
    half = 8
    inv = 500000.0 ** (-np.arange(half, dtype=np.float32) * 2.0 / 16) if False else None
    return c


def rope_tables(S):
    half = 16
    inv = (500000.0 ** (-np.arange(half, dtype=np.float32) * 2.0 / 32.0)).astype(np.float32)
    ang = np.arange(S, dtype=np.float32)[None, :] * inv[:, None]
    cos = np.cos(ang).astype(np.float32); sin = np.sin(ang).astype(np.float32)
    ropeC = np.concatenate([cos, cos], axis=0)
    ropeS = np.concatenate([-sin, sin], axis=0)
    return np.ascontiguousarray(ropeC), np.ascontiguousarray(ropeS)


def head_weights(w_in, h):
    def cols(g, a, b):
        return w_in[:, OFF[g] + a:OFF[g] + b]
    hs = h * 128
    qa = cols(0, hs, hs + 128); ka = cols(1, hs, hs + 128); va = cols(2, hs, hs + 128)
    qd = cols(3, hs, hs + 128); kd = cols(4, hs, hs + 128); vd = cols(5, hs, hs + 128); zd = cols(6, hs, hs + 128)
    qas = np.concatenate([qa[:, 16:32], qa[:, 0:16]], axis=1)
    kas = np.concatenate([ka[:, 16:32], ka[:, 0:16]], axis=1)
    be = cols(7, h, h + 1); aa = cols(8, h, h + 1)
    W1 = np.concatenate([qa, ka, qd, kd, vd, zd, qas, kas, va, be, aa], axis=1)
    assert W1.shape[1] == 962
    return np.ascontiguousarray(W1.astype(np.float32))


def head_small(inputs, h):
    sm = np.zeros((128, 24), np.float32)
    sm[:, 0:8] = nl(inputs['norm_mix_pre'][0])
    cw = np.asarray(inputs['conv_w'][0])
    hs = h * 128
    for i in range(3):
        sm[:, 8 + 4 * i:12 + 4 * i] = cw[:, i * 1024 + hs:i * 1024 + hs + 128].T
    sm[:, 20] = inputs['a_log'][0][h]
    sm[:, 21] = inputs['dt_bias'][0][h]
    sm[:, 22] = inputs['o_norm_w'][0]
    return sm


def nrm2_of(inputs):
    return np.ascontiguousarray(np.concatenate([nl(inputs['norm_mix_pre'][0]), nl(inputs['norm_mix_post'][0]),
                                                nl(inputs['norm_ffn_pre'][0]), nl(inputs['norm_ffn_post'][0])], axis=1))


I32 = mybir.dt.int32
OFF_WG, OFF_OA, OFF_OD, OFF_OUT, OFF_GATE, OFF_UP, OFF_DOWN, OFF_ROPE = (0, 262144, 393216, 524288, 655360, 1015808,
                                                                        1376256, 1736704)
WINFO = {'w_g': (OFF_WG, 2048), 'w_oa': (OFF_OA, 1024), 'w_od': (OFF_OD, 1024), 'w_out': (OFF_OUT, 1024),
         'w_gate': (OFF_GATE, 2816), 'w_up': (OFF_UP, 2816)}


def build_program(S, dbg=False):
    TPC = S // 8
    NPC = TPC // CH
    NCHT = S // CH
    SH = OFF_ROPE
    SHC = SH // 128
    nc = bass.Bass("TRN2", target_bir_lowering=False)
    dr = {}

    def din(name, shape, dt=F32):
        dr[name] = nc.dram_tensor(name, shape, dt, kind="ExternalInput").ap()

    def dint(name, shape, dt=F32):
        dr[name] = nc.dram_tensor(name, shape, dt, kind="Internal").ap()
    din('xT2', [NPC, 1024, CH]); din('wsh', [128, SHC]); din('W1', [1024, NC1]); din('sm', [128, 24]); din('delta', [128, 8]); din('rsh', [64, TPC])
    din('nrm2', [128, 32])
    din('identb', [128, 128], BF16); din('ident32', [128, 128]); din('ones', [128, 128], BF16); din('ones32', [128, 128])
    din('cau', [128, 4, 512], BF16); din('selc', [128, 383]); din('dnm', [128, 9, 128]); din('dmask', [128, 7, 128], BF16)
    dint('wi', [128, SHC]); dint('WG', [1024, SHC]); dint('xi', [NPC, 1024, CH]); dint('ri', [64, TPC]); dint('RGt', [8 * 64, TPC])
    for j in range(NPC):
        dint('gx%d' % j, [8 * 1024, CH])
    dint('yb2', [NPC, 8 * 2048, CH], BF16); dint('YR', [NPC, 2048, CH], BF16)
    dr['outT'] = nc.dram_tensor('outT', [NPC, 1024, CH], F32, kind="ExternalOutput").ap()
    WG8 = dr['WG'].rearrange("(r a) b -> r (a b)", r=8)
    RG = [list(range(8))]

    with contextlib.ExitStack() as outer:
        C = {}
        for n, dt in (('identb', BF16), ('ident32', F32), ('ones', BF16), ('ones32', F32)):
            C[n] = outer.enter_context(nc.sbuf_tensor('c_' + n, [128, 128], dt))
        with contextlib.ExitStack() as st1:
            p = Prog(nc, st1)
            tC = Tok('C')

            def ldc(e, s):
                for n in ('identb', 'ident32', 'ones', 'ones32'):
                    e.dma_start(out=C[n][:], in_=dr[n]).then_inc(s, 16)
            p.dma('sp', ldc, tC, 4, writes=[tC])
            C['t'] = tC
            delta = p.sb('delta', [128, 8], F32); t_delta = Tok('delta')
            p.dma('sp', lambda e, s: e.dma_start(out=delta[:], in_=dr['delta']).then_inc(s, 16), t_delta, 1, writes=[t_delta])
            stg = [p.sb('stg%d' % i, [128, 2, CH], BF16) for i in range(2)]
            t_stg = [Tok('stg0'), Tok('stg1')]
            stg_i = [0]
            t_xi = [Tok('xi%d' % j) for j in range(NPC)]
            t_gx = [Tok('gx%d' % j) for j in range(NPC)]
            t_wi = Tok('wi'); t_WG = Tok('WG')
            p.dma('sp', lambda e, s: e.dma_start(out=dr['xi'][0], in_=dr['xT2'][0]).then_inc(s, 16), t_xi[0], 1, writes=[t_xi[0]])
            p.cc(lambda e: e.collective_compute("AllGather", ALU.bypass, replica_groups=RG, ins=[dr['xi'][0]], outs=[dr['gx0']]),
                 reads=[t_xi[0]], writes=[t_gx[0]])
            t_ri = Tok('ri'); t_RG = Tok('RGt')
            p.dma('sp', lambda e, s: e.dma_start(out=dr['ri'], in_=dr['rsh']).then_inc(s, 16), t_ri, 1, writes=[t_ri])
            p.cc(lambda e: e.collective_compute("AllGather", ALU.bypass, replica_groups=RG, ins=[dr['ri']], outs=[dr['RGt']]),
                 reads=[t_ri], writes=[t_RG])
            p.dma('sp', lambda e, s: e.dma_start(out=dr['wi'], in_=dr['wsh']).then_inc(s, 16), t_wi, 1, writes=[t_wi])
            p.cc(lambda e: e.collective_compute("AllGather", ALU.bypass, replica_groups=RG, ins=[dr['wi']], outs=[dr['WG']]),
                 reads=[t_wi], writes=[t_WG])
            for j in range(1, NPC):
                p.dma('sp', lambda e, s, j=j: e.dma_start(out=dr['xi'][j], in_=dr['xT2'][j]).then_inc(s, 16),
                      t_xi[j], 1, writes=[t_xi[j]])
                p.cc(lambda e, j=j: e.collective_compute("AllGather", ALU.bypass, replica_groups=RG,
                                                         ins=[dr['xi'][j]], outs=[dr['gx%d' % j]]),
                     reads=[t_xi[j]], writes=[t_gx[j]])
            t_yb = [[] for _ in range(NPC)]

            class Src1:
                def x_chunk(self, t):
                    j, r = t // 8, t % 8
                    return dr['gx%d' % j][r * 1024:(r + 1) * 1024, :].rearrange("(k p) c -> p k c", p=128)

                def x_toks(self, t):
                    return [t_gx[t // 8]]

                def rope(self, t, which):
                    j, r = t // 8, t % 8
                    o = r * 64 + which * 32
                    return dr['RGt'][o:o + 32, j * CH:(j + 1) * CH]

                def rope_toks(self, t):
                    return [t_RG]

                def store_y(self, p_, t, a, tile_, tok_):
                    j, c = t // 8, t % 8
                    dst = dr['yb2'][j, c * 2048:(c + 1) * 2048, :].rearrange("(h a p) c -> p h a c", h=8, a=2, p=128)
                    for g in range(4):
                        i = stg_i[0]
                        stg_i[0] = 1 - i
                        for u in range(2):
                            hh = 2 * g + u
                            p_.op('pool', lambda e, i=i, u=u, hh=hh, tile_=tile_: e.tensor_scalar(
                                out=stg[i][:, u, :], in0=tile_[:], scalar1=delta[:, hh:hh + 1], scalar2=0.0, op0=ALU.mult, op1=ALU.add),
                                [tok_, t_delta], [t_stg[i]])
                        tk = Tok('yb2s')
                        t_yb[j].append(tk)
                        p_.dma('sp', lambda e, s, i=i, g=g, dst=dst, a=a: e.dma_start(
                            out=dst[:, 2 * g:2 * g + 2, a, :], in_=stg[i][:]).then_inc(s, 16),
                            t_stg[i], 1, reads=[t_stg[i]], writes=[tk])

                def after_chunk(self, t):
                    j, c = t // 8, t % 8
                    if c == 7:
                        p.cc(lambda e, j=j: e.collective_compute("ReduceScatter", ALU.add, replica_groups=RG,
                                                                 ins=[dr['yb2'][j]], outs=[dr['YR'][j]]),
                             reads=t_yb[j], writes=[Tok('YR%d' % j)])
            phase1(p, nc, dr, C, Src1(), S, NCHT, dbg=None)
            p.drain('sp')
            p.emit()
            stats1 = dict(p.stats)
        with contextlib.ExitStack() as st2:
            p = Prog(nc, st2)
            C['t'] = Tok('C2')

            class Src2:
                def x2_chunk(self, c):
                    return dr['xT2'][c].rearrange("(k p) c -> p k c", p=128)

                def out_chunk(self, c):
                    return dr['outT'][c].rearrange("(k p) c -> p k c", p=128)

                def load_y(self, p_, c, ya, t_ya, yd, t_yd):
                    srcv = dr['YR'][c].rearrange("(h a p) c -> p h a c", h=8, a=2, p=128)
                    for (tile_, tok_, a) in ((ya, t_ya, 0), (yd, t_yd, 1)):
                        p_.dma('sp', lambda e, s, tile_=tile_, a=a: e.dma_start(out=tile_[:], in_=srcv[:, :, a, :]).then_inc(s, 16),
                               tok_, 1, writes=[tok_])

                def w(self, name):
                    if name == 'w_down':
                        def f(dst, c0, ncols):
                            srcs = []
                            for mt in range(ncols // 128):
                                r = c0 // 128 + mt
                                srcs.append((dst[:, :, mt * 128:(mt + 1) * 128],
                                             WG8[r, OFF_DOWN:OFF_DOWN + 2816 * 128].rearrange("(f p c) -> p f c", p=128, c=128)))
                            return srcs, []
                        return f
                    off, N = WINFO[name]

                    def f(dst, c0, ncols):
                        return [(dst, WG8[:, off:off + 128 * N].rearrange("k (p n) -> p k n", n=N)[:, :, c0:c0 + ncols])], []
                    return f
            stores = phase2(p, nc, dr, C, Src2(), NPC)
            fin = p.op('sp', None)
            fin.deps = stores
            p.emit()
            stats2 = dict(p.stats)
    return nc, (stats1, stats2)


def make_in_maps(inputs, S):
    TPC = S // 8
    NPC = TPC // CH
    x = np.asarray(inputs['x'], np.float32)[0][:S]
    w_in = np.asarray(inputs['w_in'], np.float32)[0]
    cst = make_consts()
    rC, rS = rope_tables(S)
    nrm2 = nrm2_of(inputs)
    w_g = w_in[:, 7184:9232]
    mats = [w_g, np.asarray(inputs['w_o_attn'], np.float32)[0], np.asarray(inputs['w_o_delta'], np.float32)[0],
            np.asarray(inputs['w_out'], np.float32)[0], np.asarray(inputs['w_gate'], np.float32)[0],
            np.asarray(inputs['w_up'], np.float32)[0]]
    w_down = np.asarray(inputs['w_down'], np.float32)[0]
    xT = x.T
    maps = []
    for c in range(8):
        toks = np.concatenate([np.arange((8 * j + c) * CH, (8 * j + c + 1) * CH) for j in range(NPC)])
        xT2 = np.ascontiguousarray(xT[:, toks].reshape(1024, NPC, CH).transpose(1, 0, 2))
        parts = [m[c * 128:(c + 1) * 128, :].reshape(-1) for m in mats]
        parts.append(np.ascontiguousarray(w_down[:, c * 128:(c + 1) * 128]).reshape(-1))
        wsh = np.concatenate(parts).astype(np.float32)
        delta = np.zeros((128, 8), np.float32)
        delta[:, c] = 1.0
        m = {'xT2': xT2, 'W1': head_weights(w_in, c), 'sm': head_small(inputs, c), 'wsh': wsh.reshape(128, -1),
             'delta': delta, 'nrm2': nrm2,
             'rsh': np.ascontiguousarray(np.concatenate([rC[:, toks], rS[:, toks]], axis=0))}
        for k in ('identb', 'ident32', 'ones', 'ones32', 'cau', 'selc', 'dnm', 'dmask'):
            m[k] = cst[k]
        maps.append(m)
    return maps


_CACHE = {}


def kernel(**inputs):
    S = int(np.asarray(inputs['x']).shape[1])
    TPC = S // 8
    NPC = TPC // CH
    if S not in _CACHE:
        _CACHE[S] = build_program(S)[0]
    nc = _CACHE[S]
    maps = make_in_maps(inputs, S)
    res = run_bass_kernel_spmd(nc, maps, core_ids=list(range(8)))
    out = np.empty((1, S, 1024), np.float32)
    for c in range(8):
        o = np.asarray(res.results[c]['outT'], np.float32)
        for j in range(NPC):
            t = 8 * j + c
            out[0, t * CH:(t + 1) * CH, :] = o[j].T
    return out
```
